# Optimizing a Trainium2 kernel written in Bass

```python
import math
import jax, jax.numpy as jnp
from jax import lax
import numpy as np

D_MODEL = 2048
BATCH = 2
SEQ = 8192
DEPTH = 4

HEAD_DIM = 128
FOX_HEADS = D_MODEL // (2 * HEAD_DIM)
DIFF_HEADS = D_MODEL // (4 * HEAD_DIM)
FOX_WIDTH = FOX_HEADS * HEAD_DIM
DIFF_QK_WIDTH = 2 * DIFF_HEADS * HEAD_DIM
DIFF_V_WIDTH = DIFF_HEADS * 2 * HEAD_DIM
MIX_WIDTH = FOX_WIDTH + DIFF_V_WIDTH
IN_WIDTH = 3 * FOX_WIDTH + FOX_HEADS + 2 * DIFF_QK_WIDTH + DIFF_V_WIDTH
ROPE_THETA = 500000.0
ROPE_DIM = HEAD_DIM // 4
Q_BLOCK = 128
N_GROUPS = 4
EXPERTS_PER_GROUP = 8
N_EXPERTS = N_GROUPS * EXPERTS_PER_GROUP
TOP_K = 2
D_EXPERT = D_MODEL // 4
DISPATCH_CHUNK = 256
DEEPNORM_ALPHA = (2.0 * DEPTH) ** 0.25
DEEPNORM_BETA = (8.0 * DEPTH) ** -0.25
LN_EPS = 1e-5
RMS_EPS = 1e-5

kernel_name = "fox_diffattn_hiermoe_deepnorm_trunk"


def _layer_norm(x, g, b):
    xf = x.astype(jnp.float32)
    mu = jnp.mean(xf, axis=-1, keepdims=True)
    var = jnp.mean(jnp.square(xf - mu), axis=-1, keepdims=True)
    y = (xf - mu) * lax.rsqrt(var + LN_EPS)
    return (y * g.astype(jnp.float32) + b.astype(jnp.float32)).astype(x.dtype)


def _rms_norm(x, g):
    xf = x.astype(jnp.float32)
    y = xf * lax.rsqrt(jnp.mean(jnp.square(xf), axis=-1, keepdims=True) + RMS_EPS)
    return (y * g.astype(jnp.float32)).astype(x.dtype)


def _rope_tables(seq):
    pos = jnp.arange(seq, dtype=jnp.float32)
    inv_freq = ROPE_THETA ** (-jnp.arange(0, ROPE_DIM, 2, dtype=jnp.float32) / ROPE_DIM)
    ang = pos[:, None] * inv_freq[None, :]
    return jnp.cos(ang), jnp.sin(ang)


def _partial_rope(x, cos, sin):
    xr, xp = x[..., :ROPE_DIM], x[..., ROPE_DIM:]
    x1, x2 = jnp.split(xr, 2, axis=-1)
    c = cos.astype(x.dtype)
    s = sin.astype(x.dtype)
    rot = jnp.concatenate([x1 * c - x2 * s, x2 * c + x1 * s], axis=-1)
    return jnp.concatenate([rot, xp], axis=-1)


def _to_qblocks(a):
    b, h, s = a.shape[:3]
    return jnp.moveaxis(a.reshape(b, h, s // Q_BLOCK, Q_BLOCK, *a.shape[3:]), 2, 0)


def _from_qblocks(o):
    nb, b, h, qb, dv = o.shape
    return jnp.moveaxis(o, 0, 2).reshape(b, h, nb * qb, dv)


def _fox_attention(q, k, v, log_f):
    seq = q.shape[2]
    scale = HEAD_DIM ** -0.5
    cum = jnp.cumsum(log_f, axis=-1)
    kpos = jnp.arange(seq)
    qpos = kpos.reshape(seq // Q_BLOCK, Q_BLOCK)

    def block(args):
        q_b, cum_b, pos_b = args
        s = jnp.einsum('bhqd,bhkd->bhqk', q_b, k).astype(jnp.float32) * scale
        s = s + cum_b[..., :, None] - cum[:, :, None, :]
        s = jnp.where(kpos[None, :] <= pos_b[:, None], s, -jnp.inf)
        p = jax.nn.softmax(s, axis=-1)
        return jnp.einsum('bhqk,bhkd->bhqd', p.astype(v.dtype), v)

    return _from_qblocks(lax.map(block, (_to_qblocks(q), _to_qblocks(cum), qpos)))


def _diff_attention(q1, q2, k1, k2, v, lam):
    seq = q1.shape[2]
    scale = HEAD_DIM ** -0.5
    kpos = jnp.arange(seq)
    qpos = kpos.reshape(seq // Q_BLOCK, Q_BLOCK)

    def block(args):
        q1_b, q2_b, pos_b = args
        mask = kpos[None, :] <= pos_b[:, None]
        s1 = jnp.einsum('bhqd,bhkd->bhqk', q1_b, k1).astype(jnp.float32) * scale
        s2 = jnp.einsum('bhqd,bhkd->bhqk', q2_b, k2).astype(jnp.float32) * scale
        p1 = jax.nn.softmax(jnp.where(mask, s1, -jnp.inf), axis=-1)
        p2 = jax.nn.softmax(jnp.where(mask, s2, -jnp.inf), axis=-1)
        a = p1 - lam * p2
        return jnp.einsum('bhqk,bhkd->bhqd', a.astype(v.dtype), v)

    return _from_qblocks(lax.map(block, (_to_qblocks(q1), _to_qblocks(q2), qpos)))


def _hybrid_mixer(h, w_in, b_f, lam_vecs, subln_g, w_o, cos, sin, lambda_init):
    bsz, seq, _ = h.shape
    proj = h @ w_in
    o1 = FOX_WIDTH
    o2 = o1 + FOX_WIDTH
    o3 = o2 + FOX_WIDTH
    o4 = o3 + FOX_HEADS
    o5 = o4 + DIFF_QK_WIDTH
    o6 = o5 + DIFF_QK_WIDTH
    fq, fk, fv, ff = proj[..., :o1], proj[..., o1:o2], proj[..., o2:o3], proj[..., o3:o4]
    dq, dk, dv = proj[..., o4:o5], proj[..., o5:o6], proj[..., o6:]

    def heads(a, n):
        return a.reshape(bsz, seq, n, -1).transpose(0, 2, 1, 3)

    log_f = jax.nn.log_sigmoid(ff.astype(jnp.float32) + b_f.astype(jnp.float32)).transpose(0, 2, 1)
    fox_out = _fox_attention(heads(fq, FOX_HEADS), heads(fk, FOX_HEADS), heads(fv, FOX_HEADS), log_f)

    dq = dq.reshape(bsz, seq, DIFF_HEADS, 2, HEAD_DIM)
    dk = dk.reshape(bsz, seq, DIFF_HEADS, 2, HEAD_DIM)
    q1 = _partial_rope(dq[:, :, :, 0].transpose(0, 2, 1, 3), cos, sin)
    q2 = _partial_rope(dq[:, :, :, 1].transpose(0, 2, 1, 3), cos, sin)
    k1 = _partial_rope(dk[:, :, :, 0].transpose(0, 2, 1, 3), cos, sin)
    k2 = _partial_rope(dk[:, :, :, 1].transpose(0, 2, 1, 3), cos, sin)
    lv = lam_vecs.astype(jnp.float32)
    lam = jnp.exp(jnp.sum(lv[0] * lv[1])) - jnp.exp(jnp.sum(lv[2] * lv[3])) + lambda_init
    diff_out = _diff_attention(q1, q2, k1, k2, heads(dv, DIFF_HEADS), lam)
    diff_out = _rms_norm(diff_out, subln_g) * (1.0 - lambda_init)

    merged = jnp.concatenate([
        fox_out.transpose(0, 2, 1, 3).reshape(bsz, seq, FOX_WIDTH),
        diff_out.transpose(0, 2, 1, 3).reshape(bsz, seq, DIFF_V_WIDTH)], axis=-1)
    return merged @ w_o


def _hierarchical_moe(h, w_rg, b_rg, w_re, b_re, w_gate, w_up, w_down):
    bsz, seq, dm = h.shape
    n_tok = bsz * seq
    xt = h.reshape(n_tok, dm)
    g_logits = (xt @ w_rg).astype(jnp.float32) + b_rg.astype(jnp.float32)
    g_prob = jax.nn.softmax(g_logits, axis=-1)
    g_sel = jnp.argmax(g_logits, axis=-1)
    g_w = jnp.take_along_axis(g_prob, g_sel[:, None], axis=-1)
    e_logits = ((xt @ w_re).astype(jnp.float32) + b_re.astype(jnp.float32)).reshape(n_tok, N_GROUPS, EXPERTS_PER_GROUP)
    e_logits = jnp.take_along_axis(e_logits, g_sel[:, None, None], axis=1)[:, 0]
    e_prob = jax.nn.softmax(e_logits, axis=-1)
    top_w, top_i = lax.top_k(e_prob, TOP_K)
    top_w = top_w / jnp.sum(top_w, axis=-1, keepdims=True)
    gate = g_w * top_w
    expert_id = g_sel[:, None] * EXPERTS_PER_GROUP + top_i

    n_assign = n_tok * TOP_K
    chunk = DISPATCH_CHUNK
    n_slots = -(-n_assign // chunk) * chunk + N_EXPERTS * chunk
    n_chunks = n_slots // chunk
    eid = expert_id.reshape(n_assign).astype(jnp.int32)
    tok = jnp.repeat(jnp.arange(n_tok, dtype=jnp.int32), TOP_K)
    wts = gate.reshape(n_assign)
    order = jnp.argsort(eid)
    eid_s, tok_s, w_s = eid[order], tok[order], wts[order]
    counts = jnp.bincount(eid, length=N_EXPERTS)
    starts = jnp.cumsum(counts) - counts
    padded = (counts + chunk - 1) // chunk * chunk
    pends = jnp.cumsum(padded)
    pstarts = pends - padded
    dest = pstarts[eid_s] + (jnp.arange(n_assign, dtype=jnp.int32) - starts[eid_s])
    slot_tok = jnp.full((n_slots,), n_tok, dtype=jnp.int32).at[dest].set(tok_s)
    slot_w = jnp.zeros((n_slots,), jnp.float32).at[dest].set(w_s)
    chunk_start = jnp.arange(n_chunks, dtype=jnp.int32) * chunk
    chunk_expert = jnp.minimum(jnp.sum(chunk_start[:, None] >= pends[None, :], axis=1), N_EXPERTS - 1).astype(jnp.int32)
    x_pad = jnp.concatenate([xt, jnp.zeros((1, dm), xt.dtype)], axis=0)
    xs = x_pad[slot_tok].reshape(n_chunks, chunk, dm)

    def expert_chunk(args):
        xc, e = args
        hid = jax.nn.silu(xc @ w_gate[e]) * (xc @ w_up[e])
        return hid @ w_down[e]

    ys = lax.map(expert_chunk, (xs, chunk_expert)).reshape(n_slots, dm)
    ys = ys * slot_w[:, None].astype(ys.dtype)
    out = jnp.zeros((n_tok + 1, dm), ys.dtype).at[slot_tok].add(ys)[:n_tok]
    return out.reshape(bsz, seq, dm)


def setup_inputs(seed: int = 0) -> dict:
    key = jax.random.key(seed)
    ks = jax.random.split(key, 18)
    f32 = jnp.float32
    col_scale = jnp.concatenate([
        jnp.ones((2 * FOX_WIDTH,), f32), jnp.full((FOX_WIDTH,), DEEPNORM_BETA, f32),
        jnp.ones((FOX_HEADS + 2 * DIFF_QK_WIDTH,), f32), jnp.full((DIFF_V_WIDTH,), DEEPNORM_BETA, f32)])
    x = jax.random.normal(ks[0], (BATCH, SEQ, D_MODEL), f32)
    w_in = jax.random.normal(ks[1], (DEPTH, D_MODEL, IN_WIDTH), f32) * (D_MODEL ** -0.5) * col_scale
    b_f = jax.random.uniform(ks[2], (DEPTH, FOX_HEADS), f32, 1.0, 4.0)
    diff_lambda = jax.random.normal(ks[3], (DEPTH, 4, HEAD_DIM), f32) * 0.1
    diff_subln_g = 1.0 + 0.02 * jax.random.normal(ks[4], (DEPTH, 2 * HEAD_DIM), f32)
    w_o = jax.random.normal(ks[5], (DEPTH, MIX_WIDTH, D_MODEL), f32) * (MIX_WIDTH ** -0.5) * DEEPNORM_BETA
    ln1_g = 1.0 + 0.02 * jax.random.normal(ks[6], (DEPTH, D_MODEL), f32)
    ln1_b = 0.02 * jax.random.normal(ks[7], (DEPTH, D_MODEL), f32)
    w_router_group = jax.random.normal(ks[8], (DEPTH, D_MODEL, N_GROUPS), f32) * (D_MODEL ** -0.5)
    b_router_group = 0.01 * jax.random.normal(ks[9], (DEPTH, N_GROUPS), f32)
    w_router_expert = jax.random.normal(ks[10], (DEPTH, D_MODEL, N_EXPERTS), f32) * (D_MODEL ** -0.5)
    b_router_expert = 0.01 * jax.random.normal(ks[11], (DEPTH, N_EXPERTS), f32)
    w_gate = jax.random.normal(ks[12], (DEPTH, N_EXPERTS, D_MODEL, D_EXPERT), f32) * (D_MODEL ** -0.5)
    w_up = jax.random.normal(ks[13], (DEPTH, N_EXPERTS, D_MODEL, D_EXPERT), f32) * (D_MODEL ** -0.5)
    w_down = jax.random.normal(ks[14], (DEPTH, N_EXPERTS, D_EXPERT, D_MODEL), f32) * (D_EXPERT ** -0.5) * DEEPNORM_BETA
    ln2_g = 1.0 + 0.02 * jax.random.normal(ks[15], (DEPTH, D_MODEL), f32)
    ln2_b = 0.02 * jax.random.normal(ks[16], (DEPTH, D_MODEL), f32)
    return {"x": x, "w_in": w_in, "b_f": b_f, "diff_lambda": diff_lambda, "diff_subln_g": diff_subln_g,
            "w_o": w_o, "ln1_g": ln1_g, "ln1_b": ln1_b, "w_router_group": w_router_group,
            "b_router_group": b_router_group, "w_router_expert": w_router_expert,
            "b_router_expert": b_router_expert, "w_gate": w_gate, "w_up": w_up, "w_down": w_down,
            "ln2_g": ln2_g, "ln2_b": ln2_b}


def reference(x, w_in, b_f, diff_lambda, diff_subln_g, w_o, ln1_g, ln1_b, w_router_group,
              b_router_group, w_router_expert, b_router_expert, w_gate, w_up, w_down, ln2_g, ln2_b):
    cos, sin = _rope_tables(x.shape[1])
    for l in range(DEPTH):
        lambda_init = 0.8 - 0.6 * math.exp(-0.3 * l)
        mix = _hybrid_mixer(x, w_in[l], b_f[l], diff_lambda[l], diff_subln_g[l], w_o[l], cos, sin, lambda_init)
        x = _layer_norm(DEEPNORM_ALPHA * x + mix, ln1_g[l], ln1_b[l])
        ffn = _hierarchical_moe(x, w_router_group[l], b_router_group[l], w_router_expert[l],
                                b_router_expert[l], w_gate[l], w_up[l], w_down[l])
        x = _layer_norm(DEEPNORM_ALPHA * x + ffn, ln2_g[l], ln2_b[l])
    return x
```

```python
import numpy as np
import concourse.bass as bass
import concourse.mybir as mybir
from concourse.bass_utils import run_bass_kernel_spmd

F32 = mybir.dt.float32
BF16 = mybir.dt.bfloat16
I32 = mybir.dt.int32
AF = mybir.ActivationFunctionType
ALU = mybir.AluOpType
AX = mybir.AxisListType

D_MODEL = 2048
BATCH = 2
SEQ = 8192
DEPTH = 4
HEAD_DIM = 128
FOX_HEADS = 8
DIFF_HEADS = 4
FOX_WIDTH = 1024
DIFF_QK_WIDTH = 1024
DIFF_V_WIDTH = 1024
IN_WIDTH = 6152
ROPE_THETA = 500000.0
ROPE_DIM = 32
N_GROUPS = 4
EPG = 8
N_EXPERTS = 32
D_EXPERT = 512
ALPHA = (2.0 * DEPTH) ** 0.25
LN_EPS = 1e-5
RMS_EPS = 1e-5
SCALE = HEAD_DIM ** -0.5

ENG = ("pe", "act", "dve", "pool", "sp")


class Buf:
    __slots__ = ("name", "w", "r", "dsem", "dcount")

    def __init__(self, name=""):
        self.name = name
        self.w = None
        self.r = []
        self.dsem = None
        self.dcount = 0


class Prog:
    def __init__(self):
        self.ops = {e: [] for e in ENG}
        self.waited = {e: {} for e in ENG}
        self.dbufs = []

    def _waits(self, eng, reads, writes):
        deps = []
        for b in reads:
            if b.w is not None:
                deps.append(b.w)
        for b in writes:
            if b.w is not None:
                deps.append(b.w)
            deps.extend(b.r)
        waits = []
        wd = self.waited[eng]
        for d in deps:
            kind, key, val = d
            if kind == "e" and key == eng and eng == "pe":
                continue
            wk = key if kind == "e" else id(key)
            if wd.get(wk, -1) >= val:
                continue
            wd[wk] = val
            waits.append(d)
            if kind == "e":
                self.ops[key][val]["ms"] = True
        return waits

    def op(self, eng, meth, *args, reads=(), writes=(), **kw):
        px = [b for b in reads if b.name.startswith("bank")]
        if px:
            reads = [b for b in reads if not b.name.startswith("bank")]
            writes = list(writes) + px
        waits = self._waits(eng, reads, writes)
        idx = len(self.ops[eng])
        fn = (meth, args, kw)
        self.ops[eng].append(dict(waits=waits, fn=fn, ms=False, dma=None))
        tok = ("e", eng, idx)
        for b in reads:
            b.r.append(tok)
        for b in writes:
            b.w = tok
            b.r = []
        return tok

    def dma(self, eng, sembuf, out, in_, reads=(), writes=(), **kw):
        waits = self._waits(eng, reads, writes)
        kw = dict(kw)
        kw.update(out=out, in_=in_)
        fn = (kw.pop("meth", "dma_start"), (), kw)
        if sembuf.dsem is None:
            sembuf.dsem = {}
        if eng not in sembuf.dsem:
            sembuf.dsem[eng] = Buf("dsem")
            self.dbufs.append(sembuf.dsem[eng])
        ds = sembuf.dsem[eng]
        ds.dcount += 16
        self.ops[eng].append(dict(waits=waits, fn=fn, ms=False, dma=ds))
        tok = ("d", ds, ds.dcount)
        for b in reads:
            b.r.append(tok)
        for b in writes:
            b.w = tok
            b.r = []
        return tok

    def coll(self, eng, sembuf, kind, alu, groups, ins, outs, reads=(), writes=()):
        waits = self._waits(eng, reads, writes)
        fn = ("collective_compute", (kind, alu), dict(replica_groups=groups, ins=ins, outs=outs))
        if sembuf.dsem is None:
            sembuf.dsem = {}
        key = eng + "_cc"
        if key not in sembuf.dsem:
            sembuf.dsem[key] = Buf("dsem")
            self.dbufs.append(sembuf.dsem[key])
        ds = sembuf.dsem[key]
        ds.dcount += 1
        self.ops[eng].append(dict(waits=waits, fn=fn, ms=False, dma=ds, inc=1))
        tok = ("d", ds, ds.dcount)
        for b in reads:
            b.r.append(tok)
        for b in writes:
            b.w = tok
            b.r = []
        return tok

    def final_wait(self, eng, bufs):
        waits = self._waits(eng, bufs, ())
        self.ops[eng].append(dict(waits=waits, fn=None, ms=False, dma=None))

    def emit(self, nc, stack):
        esem = {}
        for e in ENG:
            esem[e] = stack.enter_context(nc.semaphore("s_" + e))
        for i, b in enumerate(self.dbufs):
            b.dsem = stack.enter_context(nc.semaphore("d%d" % i))
        msnum = {}
        for e in ENG:
            n = 0
            arr = []
            for o in self.ops[e]:
                if o["ms"]:
                    n += 1
                arr.append(n)
            msnum[e] = arr
        block = stack.enter_context(nc.Block())
        starters = dict(pe=block.tensor, act=block.scalar, dve=block.vector,
                        pool=block.gpsimd, sp=block.sync)

        def run(ename):
            def body(eng):
                for o in self.ops[ename]:
                    for kind, key, val in o["waits"]:
                        if kind == "e":
                            eng.wait_ge(esem[key], msnum[key][val])
                        else:
                            eng.wait_ge(key.dsem, val)
                    if o["fn"] is None:
                        continue
                    meth, args, kw = o["fn"]
                    ins = getattr(eng, meth)(*args, **kw)
                    if o["dma"] is not None:
                        ins.then_inc(o["dma"].dsem, o.get("inc", 16))
                    elif o["ms"]:
                        ins.then_inc(esem[ename], 1)
            return body

        for e in ENG:
            if self.ops[e]:
                starters[e](run(e))


FOXW = 386
DIFFW = 896
NWC = 2 * FOXW + DIFFW
MISC = 4 + 512 + 256


def build_A(S, stage=99, ctx=None):
    from contextlib import ExitStack
    NT = S // 512
    NB = S // 128
    if ctx is None:
        nc = bass.Bass("TRN2", target_bir_lowering=False)
        xT_d = nc.dram_tensor("xT", [2048, S], F32, kind="ExternalInput").ap()
        wc_d = nc.dram_tensor("wc", [2048, NWC], F32, kind="ExternalInput").ap()
        cc_d = nc.dram_tensor("cc", [32, S], F32, kind="ExternalInput").ap()
        ss_d = nc.dram_tensor("ss", [32, S], F32, kind="ExternalInput").ap()
        misc_d = nc.dram_tensor("misc", [1, MISC], F32, kind="ExternalInput").ap()
        tri_d = nc.dram_tensor("tri", [128, 128], F32, kind="ExternalInput").ap()
        mask_d = nc.dram_tensor("mask", [128, 128], F32, kind="ExternalInput").ap()
        out_d = nc.dram_tensor("merged", [S, 512], F32, kind="ExternalOutput").ap()
        P = Prog()
        st = ExitStack()
        sb = lambda name, shape, dt: st.enter_context(nc.sbuf_tensor(name, shape, dt))
    else:
        nc = ctx["nc"]
        xT_d, wc_d, cc_d, ss_d, misc_d, tri_d, mask_d = (ctx[k] for k in ("xT", "wc", "cc", "ss", "misc", "tri", "mask"))
        out_d = None
        P = ctx["P"]
        st = None
        sb = ctx["alloc"]
    W = sb("W", [128, 16, DIFFW], BF16)
    xt = [sb("xt%d" % i, [128, 16, 512], BF16) for i in range(2)]
    QT = [sb("QT%d" % i, [128, S], BF16) for i in range(2)]
    KT = [sb("KT%d" % i, [128, S], BF16) for i in range(2)]
    VA = sb("VA", [128, NB, 2, 129], BF16)
    PT = [sb("PT%d" % i, [128, 512], BF16) for i in range(3)]
    CCt = [sb("CCt%d" % i, [128, 512], F32) for i in range(2)]
    SSt = [sb("SSt%d" % i, [128, 512], F32) for i in range(2)]
    R1 = sb("R1", [128, 512], F32)
    R2 = sb("R2", [128, 512], F32)
    FF = sb("FF", [128, NB], F32)
    LL = sb("LL", [128, NB], F32)
    W1 = sb("W1", [128, NB], F32)
    INCL = sb("INCL", [128, NB], F32)
    NEGC = sb("NEGC", [128, NB], F32)
    ONESR = sb("ONESR", [128, NB], F32)
    OS = [sb("OS%d" % i, [128, 4, 256], F32) for i in range(2)]
    O1 = sb("O1", [128, 4, 256], F32)
    O2 = sb("O2", [128, 4, 256], F32)
    DD = sb("DD", [128, 256], F32)
    SQ = sb("SQ", [128, 256], F32)
    SM = sb("SM", [128, 16], F32)
    MISCt = sb("MISCt", [128, MISC], F32)
    G2 = sb("G2", [128, 256], F32)
    TRI = sb("TRI", [128, 128], F32)
    ONES = sb("ONES", [128, 128], F32)
    MASKf = sb("MASKf", [128, 128], F32)
    MASK = sb("MASK", [128, 128], BF16)
    ZB = sb("ZB", [128, 1], F32)
    if ctx is None:
        banks = [st.enter_context(nc.psum_tensor("bk%d" % i, [128, 512], F32)) for i in range(8)]
        BIAS = KT[1].bitcast(F32)
    else:
        banks = ctx["banks"]
        BIAS = ctx["alias_f32"](KT[1], S // 2)
        IDENTA = sb("IDENTA", [128, 128], F32)
        OT = [sb("OT%d" % i, [128, 512], F32) for i in range(2)]
        bOT = [Buf(), Buf()]

    bW = Buf("W")
    bxt = [Buf(), Buf()]
    bQT = [[Buf() for _ in range(NT)] for _ in range(2)]
    bKT = [[Buf() for _ in range(NT)] for _ in range(2)]
    bVA = [Buf() for _ in range(NT)]
    bPT = [[Buf() for _ in range(4)] for _ in range(3)]
    bCS = [Buf(), Buf()]
    bR1, bR2 = Buf(), Buf()
    bFF, bLL, bW1, bINCL, bNEGC, bBIAS = Buf(), Buf(), Buf(), Buf(), Buf(), Buf()
    bOS = [Buf(), Buf()]
    bO1, bO2, bDD, bSQ, bSM = Buf(), Buf(), Buf(), Buf(), Buf()
    bMISC, bG2, bCONST = Buf(), Buf(), Buf()
    bbk = [Buf("bank%d" % i) for i in range(8)]
    bOUT = Buf("out")

    P.dma("sp", bMISC, MISCt[:, :], misc_d.partition_broadcast(128), writes=[bMISC])
    P.dma("sp", bCONST, TRI[:, :], tri_d, writes=[bCONST])
    P.dma("sp", bCONST, MASKf[:, :], mask_d, writes=[bCONST])
    P.op("dve", "memset", ONES[:, :], 1.0, writes=[bCONST])
    P.op("dve", "memset", ONESR[:, :], 1.0, writes=[bCONST])
    P.op("dve", "tensor_copy", MASK[:, :], MASKf[:, :], reads=[bCONST], writes=[bCONST])
    P.op("dve", "memset", VA[:, :, :, 128:129], 1.0, writes=bVA)
    P.op("dve", "tensor_scalar", SM[:, 0:2], MISCt[:, 0:2], -1.0, None, ALU.mult, reads=[bMISC], writes=[bSM])
    P.op("dve", "tensor_tensor", SQ[:, 0:128], MISCt[:, 4:132], MISCt[:, 132:260], ALU.mult,
         reads=[bMISC], writes=[bSQ])
    P.op("dve", "tensor_tensor", SQ[:, 128:256], MISCt[:, 260:388], MISCt[:, 388:516], ALU.mult,
         reads=[bMISC, bSQ], writes=[bSQ])
    P.op("dve", "reduce_sum", SM[:, 2:3], SQ[:, 0:128], AX.X, reads=[bSQ, bSM], writes=[bSM])
    P.op("dve", "reduce_sum", SM[:, 3:4], SQ[:, 128:256], AX.X, reads=[bSQ, bSM], writes=[bSM])
    P.op("act", "activation", SM[:, 4:6], SM[:, 2:4], AF.Exp, reads=[bSM], writes=[bSM])
    P.op("dve", "tensor_tensor", SM[:, 6:7], SM[:, 4:5], SM[:, 5:6], ALU.subtract, reads=[bSM], writes=[bSM])
    P.op("dve", "tensor_tensor", SM[:, 6:7], SM[:, 6:7], MISCt[:, 2:3], ALU.add, reads=[bSM, bMISC], writes=[bSM])
    P.op("dve", "tensor_scalar", SM[:, 7:8], SM[:, 6:7], -1.0, None, ALU.mult, reads=[bSM], writes=[bSM])
    P.op("dve", "tensor_scalar", G2[:, :], MISCt[:, 516:772], MISCt[:, 3:4], None, ALU.mult,
         reads=[bMISC], writes=[bG2])

    bank_rr = [0]

    def next_bank():
        i = bank_rr[0]
        bank_rr[0] = (i + 1) % 8
        return i

    xT_v = xT_d.rearrange("(c p) t -> p c t", p=128)
    wc_v = wc_d.rearrange("(c p) n -> p c n", p=128)

    def load_W(base, width):
        for c4 in range(4):
            P.dma("pool", bW, W[:, c4 * 4:(c4 + 1) * 4, 0:width],
                  wc_v[:, c4 * 4:(c4 + 1) * 4, base:base + width], writes=[bW])

    def load_xt(t):
        i = t % 2
        for c4 in range(4):
            P.dma("pool", bxt[i], xt[i][:, c4 * 4:(c4 + 1) * 4, :],
                  xT_v[:, c4 * 4:(c4 + 1) * 4, t * 512:(t + 1) * 512], writes=[bxt[i]])

    def load_cs(t):
        i = t % 2
        P.dma("sp", bCS[i], CCt[i][0:32, :], cc_d[:, t * 512:(t + 1) * 512], writes=[bCS[i]])
        P.dma("sp", bCS[i], SSt[i][0:32, :], ss_d[:, t * 512:(t + 1) * 512], writes=[bCS[i]])

    cp_rr = [0]

    def evac(out_ap, in_ap, reads, writes):
        cp_rr[0] ^= 1
        if cp_rr[0]:
            P.op("act", "activation", out_ap, in_ap, AF.Copy, reads=reads, writes=writes)
        else:
            P.op("dve", "tensor_copy", out_ap, in_ap, reads=reads, writes=writes)

    def proj_T(t, woff, m, dst, dbuf, extra_w=()):
        bi = next_bank()
        bk = banks[bi]
        for c in range(16):
            P.op("pe", "matmul", bk[0:m, :], lhsT=W[:, c, woff:woff + m], rhs=xt[t % 2][:, c, :],
                 start=(c == 0), stop=(c == 15), reads=[bW, bxt[t % 2]], writes=[bbk[bi]])
        if dst is not None:
            evac(dst[0:m, t * 512:(t + 1) * 512], bk[0:m, :], [bbk[bi]], [dbuf] + list(extra_w))
        return bi

    def proj_V(t, voff, vw, nunits, with_ff):
        wtot = vw + (1 if with_ff else 0)
        for half in range(2):
            bi = next_bank()
            bk = banks[bi]
            for s2 in range(2):
                s = half * 2 + s2
                for c in range(16):
                    P.op("pe", "matmul", bk[:, s2 * wtot:(s2 + 1) * wtot],
                         lhsT=xt[t % 2][:, c, s * 128:(s + 1) * 128], rhs=W[:, c, voff:voff + wtot],
                         start=(c == 0), stop=(c == 15), reads=[bW, bxt[t % 2]], writes=[bbk[bi]])
            for s2 in range(2):
                s = half * 2 + s2
                blk = t * 4 + s
                src = bk[:, s2 * wtot:s2 * wtot + vw]
                if nunits == 2:
                    src = src.rearrange("p (u d) -> p u d", u=2)
                    dstv = VA[:, blk, :, 0:128]
                else:
                    dstv = VA[:, blk, 0, 0:128]
                evac(dstv, src, [bbk[bi]], [bVA[t]])
                if with_ff:
                    P.op("dve", "tensor_copy", FF[:, blk:blk + 1], bk[:, s2 * wtot + vw:s2 * wtot + vw + 1],
                         reads=[bbk[bi]], writes=[bFF])

    def attention(maps, nunits, bias, Qb, finish):
        nkb = 4 * (Qb + 1)
        for mi, m in enumerate(maps):
            aset = (Qb * len(maps) + mi) % 2
            abanks = [aset * 3 + j for j in range(3)]
            nacc = 4 * nunits

            def acc_ap(s, u, abanks=abanks):
                a = s * nunits + u
                return banks[abanks[a // 3]][:, (a % 3) * 129:(a % 3) * 129 + 129]

            def acc_b(s, u, abanks=abanks):
                a = s * nunits + u
                return bbk[abanks[a // 3]]
            for j in range((nacc + 2) // 3):
                P.op("dve", "memset", banks[abanks[j]][:, 0:387], 0.0, writes=[bbk[abanks[j]]])

            def issue_st(kb):
                sbi = 6 + (kb % 2)
                a0 = max(kb - 4 * Qb, 0)
                P.op("pe", "matmul", banks[sbi][:, a0 * 128:512], lhsT=KT[m][:, kb * 128:(kb + 1) * 128],
                     rhs=QT[m][:, Qb * 512 + a0 * 128:(Qb + 1) * 512], start=True, stop=True,
                     reads=[bKT[m][kb // 4], bQT[m][Qb]], writes=[bbk[sbi]])

            issue_st(0)
            for kb in range(nkb):
                if kb + 1 < nkb:
                    issue_st(kb + 1)
                sbi = 6 + (kb % 2)
                j = kb - 4 * Qb
                a0 = max(j, 0)
                pi = kb % 3
                if bias:
                    for s in range(a0, 4):
                        qb = 4 * Qb + s
                        P.op("act", "activation", PT[pi][:, s * 128:(s + 1) * 128],
                             banks[sbi][:, s * 128:(s + 1) * 128], AF.Exp,
                             bias=BIAS[:, kb * NB + qb:kb * NB + qb + 1], scale=SCALE,
                             reads=[bbk[sbi], bBIAS], writes=[bPT[pi][s]])
                else:
                    P.op("act", "activation", PT[pi][:, a0 * 128:512], banks[sbi][:, a0 * 128:512], AF.Exp,
                         scale=SCALE, reads=[bbk[sbi]], writes=bPT[pi][a0:4])
                if j >= 0:
                    P.op("dve", "tensor_tensor", PT[pi][:, j * 128:(j + 1) * 128],
                         PT[pi][:, j * 128:(j + 1) * 128], MASK[:, :], ALU.mult,
                         reads=[bPT[pi][j], bCONST], writes=[bPT[pi][j]])
                for s in range(a0, 4):
                    for u in range(nunits):
                        P.op("pe", "matmul", acc_ap(s, u), lhsT=PT[pi][:, s * 128:(s + 1) * 128],
                             rhs=VA[:, kb, u, :], start=False, stop=(kb == 4 * Qb + s), skip_group_check=True,
                             reads=[bPT[pi][s], bVA[kb // 4]], writes=[acc_b(s, u)])
            finish(mi, acc_ap, acc_b)

    def normalize(dst, dbuf, acc_ap, acc_b, nunits, col0):
        for s in range(4):
            for u in range(nunits):
                k = 8 + (s * 2 + u) % 8
                P.op("dve", "reciprocal", SM[:, k:k + 1], acc_ap(s, u)[:, 128:129],
                     reads=[acc_b(s, u)], writes=[bSM])
                P.op("dve", "tensor_scalar", dst[:, s, col0 + u * 128:col0 + (u + 1) * 128],
                     acc_ap(s, u)[:, 0:128], SM[:, k:k + 1], None, ALU.mult,
                     reads=[acc_b(s, u), bSM], writes=[dbuf])

    if ctx is None:
        out_v = out_d.rearrange("(n s q) c -> n q s c", s=4, q=128)
    else:
        P.dma("sp", bCONST, IDENTA[:, :], ctx["ident"], writes=[bCONST])
    ot_rr = [0]

    def write_out(Qb, osi, col0, width):
        if ctx is None:
            P.dma("sp", bOUT, out_v[Qb, :, :, col0:col0 + width], OS[osi][:, :, 0:width],
                  reads=[bOS[osi]], writes=[bOUT])
            return
        mt = ctx["MTint"]
        for u in range(width // 128):
            bi = next_bank()
            for s in range(4):
                P.op("pe", "transpose", banks[bi][:, s * 128:(s + 1) * 128], OS[osi][:, s, u * 128:(u + 1) * 128],
                     IDENTA[:, :], reads=[bOS[osi], bCONST], writes=[bbk[bi]])
            k = ot_rr[0] % 2
            ot_rr[0] += 1
            evac(OT[k][:, :], banks[bi][:, :], [bbk[bi]], [bOT[k]])
            r0 = col0 + u * 128
            P.dma("sp", bOUT, mt[r0:r0 + 128, Qb * 512:(Qb + 1) * 512], OT[k][:, :], reads=[bOT[k]], writes=[bOUT])

    def early():
        P.op("dve", "memset", OS[0][:, :, :], 0.0, writes=[bOS[0]])
        P.dma("sp", bOUT, out_v[0, :, :, 256:512], OS[0][:, :, :], reads=[bOS[0]], writes=[bOUT])
        P.final_wait("sp", [bOUT])
        P.emit(nc, st)
        st.close()
        return nc
    if stage == 0:
        return early()

    for g in range(2):
        base = g * FOXW
        load_W(base, FOXW)
        for t in range(NT):
            load_xt(t)
            proj_T(t, 0, 128, QT[0], bQT[0][t])
            proj_T(t, 128, 128, KT[0], bKT[0][t])
            proj_V(t, 256, 128, 1, True)
        if stage == 1:
            return early()
        P.op("act", "activation", LL[:, :], FF[:, :], AF.Exp, bias=SM[:, g:g + 1], scale=-1.0,
             reads=[bFF, bSM], writes=[bLL])
        P.op("act", "activation", LL[:, :], LL[:, :], AF.Ln, bias=ONES[:, 0:1], scale=1.0,
             reads=[bLL, bCONST], writes=[bLL])
        bi = next_bank()
        P.op("pe", "matmul", banks[bi][:, 0:NB], lhsT=TRI[:, :], rhs=LL[:, :], start=True, stop=True,
             reads=[bLL, bCONST], writes=[bbk[bi]])
        P.op("dve", "tensor_copy", W1[:, :], banks[bi][:, 0:NB], reads=[bbk[bi]], writes=[bW1])
        bi2 = next_bank()
        P.op("pe", "matmul", banks[bi2][:, 0:NB], lhsT=ONES[:, :], rhs=LL[:, :], start=True, stop=True,
             reads=[bLL, bCONST], writes=[bbk[bi2]])
        P.op("dve", "tensor_copy", NEGC[:, :], banks[bi2][:, 0:NB], reads=[bbk[bi2]], writes=[bNEGC])
        P.op("dve", "tensor_tensor_scan", INCL[:, :], ONESR[:, :], NEGC[:, :], 0.0, ALU.mult, ALU.add,
             reads=[bNEGC, bCONST], writes=[bINCL])
        P.op("dve", "tensor_tensor", NEGC[:, :], INCL[:, :], NEGC[:, :], ALU.subtract,
             reads=[bINCL, bNEGC], writes=[bNEGC])
        P.op("dve", "tensor_tensor", NEGC[:, :], NEGC[:, :], W1[:, :], ALU.add,
             reads=[bNEGC, bW1], writes=[bNEGC])
        for kb in range(NB):
            P.op("dve", "tensor_scalar", BIAS[:, kb * NB:(kb + 1) * NB], INCL[:, :], -1.0,
                 NEGC[:, kb:kb + 1], ALU.mult, ALU.add,
                 reads=[bINCL, bNEGC], writes=[bBIAS] + bKT[1])
        if stage == 2:
            return early()
        for Qb in range(NT):
            osi = Qb % 2

            def fin(mi, acc_ap, acc_b, Qb=Qb, osi=osi, g=g):
                normalize(OS[osi], bOS[osi], acc_ap, acc_b, 1, 0)
                write_out(Qb, osi, g * 128, 128)
            attention([0], 1, True, Qb, fin)

    if stage == 3:
        return early()
    base = 2 * FOXW
    load_W(base, DIFFW)
    for t in range(NT):
        load_xt(t)
        load_cs(t)
        i = t % 2
        for mi in range(2):
            for which, dst, dbufs in (("q", QT, bQT), ("k", KT, bKT)):
                woff = (0 if which == "q" else 256) + mi * 128
                swoff = 512 + (0 if which == "q" else 64) + mi * 32
                extra = [bBIAS] if (which == "k" and mi == 1) else []
                if stage in (4.1,):
                    continue
                bi = proj_T(t, woff, 128, dst[mi], dbufs[mi][t], extra)
                if stage in (4.2,):
                    continue
                bs = proj_T(t, swoff, 32, None, None)
                if stage in (4.3,):
                    continue
                if stage == 4.45:
                    P.op("dve", "tensor_copy", R1[0:32, :], CCt[i][0:32, :], reads=[bCS[i]], writes=[bR1])
                    continue
                if stage == 4.46:
                    P.op("dve", "tensor_copy", R1[0:32, :], banks[bi][0:32, :], reads=[bbk[bi]], writes=[bR1])
                    continue
                if stage == 4.47:
                    P.op("dve", "tensor_copy", R1[0:32, :], banks[bs][0:32, :], reads=[bbk[bs]], writes=[bR1])
                    continue
                P.op("dve", "tensor_tensor", R1[0:32, :], banks[bi][0:32, :], CCt[i][0:32, :], ALU.mult,
                     reads=[bbk[bi], bCS[i]], writes=[bR1])
                if stage == 4.41:
                    continue
                P.op("dve", "tensor_tensor", R2[0:32, :], banks[bs][0:32, :], SSt[i][0:32, :], ALU.mult,
                     reads=[bbk[bs], bCS[i]], writes=[bR2])
                P.op("dve", "tensor_tensor", dst[mi][0:32, t * 512:(t + 1) * 512], R1[0:32, :], R2[0:32, :], ALU.add,
                     reads=[bR1, bR2], writes=[dbufs[mi][t]] + extra)
        if 4 < stage < 5:
            continue
        proj_V(t, 640, 256, 2, False)
    if 4 < stage < 5:
        return early()
    if stage == 4:
        return early()
    for Qb in range(NT):
        osi = Qb % 2

        def fin(mi, acc_ap, acc_b, Qb=Qb, osi=osi):
            if mi == 0:
                normalize(O1, bO1, acc_ap, acc_b, 2, 0)
                return
            normalize(O2, bO2, acc_ap, acc_b, 2, 0)
            for s in range(4):
                P.op("dve", "scalar_tensor_tensor", DD[:, :], O2[:, s, :], SM[:, 7:8], O1[:, s, :],
                     ALU.mult, ALU.add, reads=[bO1, bO2, bSM], writes=[bDD])
                P.op("dve", "tensor_tensor", SQ[:, :], DD[:, :], DD[:, :], ALU.mult, reads=[bDD], writes=[bSQ])
                P.op("dve", "reduce_sum", SM[:, 2:3], SQ[:, :], AX.X, reads=[bSQ, bSM], writes=[bSM])
                P.op("dve", "tensor_scalar", SM[:, 3:4], SM[:, 2:3], 1.0 / 256.0, RMS_EPS, ALU.mult, ALU.add,
                     reads=[bSM], writes=[bSM])
                P.op("act", "activation", SM[:, 3:4], SM[:, 3:4], AF.Ln, reads=[bSM], writes=[bSM])
                P.op("act", "activation", SM[:, 3:4], SM[:, 3:4], AF.Exp, scale=-0.5, reads=[bSM], writes=[bSM])
                P.op("dve", "scalar_tensor_tensor", OS[osi][:, s, :], DD[:, :], SM[:, 3:4], G2[:, :],
                     ALU.mult, ALU.mult, reads=[bDD, bSM, bG2], writes=[bOS[osi]])
            write_out(Qb, osi, 256, 256)
        attention([0, 1], 2, False, Qb, fin)

    if ctx is not None:
        ctx["bMT"] = bOUT
        return None
    P.final_wait("sp", [bOUT])
    P.emit(nc, st)
    st.close()
    return nc


def rope_tables(S):
    pos = np.arange(S, dtype=np.float32)
    inv = (np.float32(ROPE_THETA) ** (-np.arange(0, ROPE_DIM, 2, dtype=np.float32) / np.float32(ROPE_DIM))).astype(np.float32)
    ang = (pos[:, None] * inv[None, :]).astype(np.float32)
    c = np.cos(ang).astype(np.float32).T
    s = np.sin(ang).astype(np.float32).T
    cc = np.concatenate([c, c], axis=0)
    ss = np.concatenate([-s, s], axis=0)
    return np.ascontiguousarray(cc), np.ascontiguousarray(ss)


def a_weight_cols(r):
    o1, o2, o3 = FOX_WIDTH, 2 * FOX_WIDTH, 3 * FOX_WIDTH
    o4 = o3 + FOX_HEADS
    o5 = o4 + DIFF_QK_WIDTH
    o6 = o5 + DIFF_QK_WIDTH
    cols = []
    ar = np.arange
    for h in (2 * r, 2 * r + 1):
        cols += list(h * 128 + ar(128))
        cols += list(o1 + h * 128 + ar(128))
        cols += list(o2 + h * 128 + ar(128))
        cols += [o3 + h, o3 + h]
    h = r
    q1 = o4 + h * 256 + ar(128)
    q2 = o4 + h * 256 + 128 + ar(128)
    k1 = o5 + h * 256 + ar(128)
    k2 = o5 + h * 256 + 128 + ar(128)
    sw = lambda b: list(b[16:32]) + list(b[0:16])
    cols += list(q1) + list(q2) + list(k1) + list(k2)
    cols += sw(q1) + sw(q2) + sw(k1) + sw(k2)
    cols += list(o6 + h * 256 + ar(256))
    assert len(cols) == NWC
    return np.array(cols)


def lambda_init(l):
    import math
    return 0.8 - 0.6 * math.exp(-0.3 * l)


def a_in_maps(xT_b, w_in_l, b_f_l, lam_l, subg_l, l, S):
    cc, ss = rope_tables(S)
    tri = np.triu(np.ones((128, 128), np.float32))
    mask = np.triu(np.ones((128, 128), np.float32))
    li = np.float32(lambda_init(l))
    maps = []
    for c in range(8):
        b, r = c // 4, c % 4
        wc = np.ascontiguousarray(w_in_l[:, a_weight_cols(r)])
        misc = np.zeros((1, MISC), np.float32)
        misc[0, 0] = b_f_l[2 * r]
        misc[0, 1] = b_f_l[2 * r + 1]
        misc[0, 2] = li
        misc[0, 3] = np.float32(1.0) - li
        misc[0, 4:516] = lam_l.reshape(-1)
        misc[0, 516:772] = subg_l
        maps.append(dict(xT=xT_b[b], wc=wc, cc=cc, ss=ss, misc=misc, tri=tri, mask=mask))
    return maps


def a_assemble(results, S):
    merged = np.zeros((BATCH, S, 2048), np.float32)
    for c in range(8):
        b, r = c // 4, c % 4
        m = results[c]["merged"]
        merged[b, :, 256 * r:256 * r + 256] = m[:, 0:256]
        merged[b, :, 1024 + 256 * r:1024 + 256 * r + 256] = m[:, 256:512]
    return merged


def _barrier(P):
    last = {e: len(P.ops[e]) - 1 for e in ENG}
    for e in ENG:
        waits = []
        for e2 in ENG:
            if e2 == e or last[e2] < 0:
                continue
            if P.waited[e].get(e2, -1) >= last[e2]:
                continue
            k = last[e2]
            while k >= 0 and (P.ops[e2][k]["fn"] is None or P.ops[e2][k]["dma"] is not None):
                k -= 1
            if k < 0:
                continue
            P.waited[e][e2] = k
            P.ops[e2][k]["ms"] = True
            waits.append(("e", e2, k))
        for b in P.dbufs:
            if P.waited[e].get(id(b), -1) >= b.dcount:
                continue
            P.waited[e][id(b)] = b.dcount
            waits.append(("d", b, b.dcount))
        P.ops[e].append(dict(waits=waits, fn=None, ms=False, dma=None))


def build_B(T, C, gather=True, stage=99, ctx=None):
    GSPACE = "Local"
    from contextlib import ExitStack
    NTT = T // 128
    NSL = C // 128
    NSLOT = 32 * C
    NROW = NSLOT + 128
    if ctx is None:
        nc = bass.Bass("TRN2", target_bir_lowering=False)
        dt_in = lambda name, shape, dt=F32: nc.dram_tensor(name, shape, dt, kind="ExternalInput").ap()
        mT_d = dt_in("mT", [2048, T])
        x_d = dt_in("x", [T, 2048])
        wo_d = dt_in("wo", [2048, 2048])
        ln_d = dt_in("ln", [1, 8192])
        wr_d = dt_in("wr", [2048, 36])
        br_d = dt_in("br", [1, 36])
        if gather:
            wgs_d = dt_in("wg", [4 * 2048, 512])
            wus_d = dt_in("wu", [4 * 2048, 512])
            wds_d = dt_in("wd", [4 * 512, 2048])
            wgi_d = nc.dram_tensor("wgi", [4 * 2048, 512], F32, kind="Internal").ap()
            wui_d = nc.dram_tensor("wui", [4 * 2048, 512], F32, kind="Internal").ap()
            wdi_d = nc.dram_tensor("wdi", [4 * 512, 2048], F32, kind="Internal").ap()
        if gather:
            wg_d = nc.dram_tensor("wgg", [32 * 2048, 512], F32, kind="Internal", addr_space=GSPACE).ap()
            wu_d = nc.dram_tensor("wug", [32 * 2048, 512], F32, kind="Internal", addr_space=GSPACE).ap()
            wd_d = nc.dram_tensor("wdg", [32 * 512, 2048], F32, kind="Internal", addr_space=GSPACE).ap()
        else:
            wg_d = dt_in("wgg", [32 * 2048, 512])
            wu_d = dt_in("wug", [32 * 2048, 512])
            wd_d = dt_in("wdg", [32 * 512, 2048])
        ident_d = dt_in("ident", [128, 128])
        tris_d = dt_in("tris", [128, 128])
        ec_d = dt_in("ec", [1, 32])
        tok_d = dt_in("tok", [128, NTT * 16], I32)
        y_d = nc.dram_tensor("y", [T, 2048], F32, kind="ExternalOutput").ap()
        X1F_d = nc.dram_tensor("X1F", [T, 2048], F32, kind="Internal").ap()
        X1H_d = nc.dram_tensor("X1H", [T, 2048], BF16, kind="Internal").ap()
        ST_d = nc.dram_tensor("ST", [NROW, 16], I32, kind="Internal").ap()
        YS_d = nc.dram_tensor("YS", [NROW, 2048], F32, kind="Internal").ap()
        P = Prog()
        st = ExitStack()
        sb = lambda name, shape, dt: st.enter_context(nc.sbuf_tensor(name, shape, dt))
    else:
        nc = ctx["nc"]
        gather = False
        (x_d, wo_d, ln_d, wr_d, br_d, wg_d, wu_d, wd_d, ident_d, tris_d, ec_d, tok_d, y_d, X1F_d, X1H_d, ST_d, YS_d) = (
            ctx[k] for k in ("x", "wo", "ln", "wr", "br", "wgg", "wug", "wdg", "ident", "tris", "ec", "tok", "y",
                             "X1F", "X1H", "ST", "YS"))
        P = ctx["P"]
        st = None
        sb = ctx["alloc"]
    BIGW = sb("BIGW", [128, 32768], BF16)
    ARENA = sb("ARENA", [128, 40960], BF16)
    ARENAf = ARENA.bitcast(F32) if ctx is None else ctx["alias_f32"](ARENA, 20480)
    LNP = sb("LNP", [128, 8192], F32)
    WR = sb("WR", [128, 16, 36], F32)
    BR = sb("BR", [128, 36], F32)
    IDENT = sb("IDENT", [128, 128], F32)
    IDENTb = sb("IDENTb", [128, 128], BF16)
    TRISf = sb("TRISf", [128, 128], F32)
    TRISb = sb("TRISb", [128, 128], BF16)
    ONESb = sb("ONESb", [128, 128], BF16)
    EC = sb("EC", [128, 32], F32)
    TOK = sb("TOK", [128, NTT * 16], I32)
    ZI = sb("ZI", [128, (NROW // 128) * 16], I32)
    LG = sb("LG", [128, 36], F32)
    SMB = sb("SMB", [128, 64], F32)
    STATS = sb("STATS", [128, 24], F32)
    T8 = sb("T8", [128, 8], F32)
    EM = sb("EM", [128, 32], F32)
    MM1 = sb("MM1", [128, NTT, 32], F32)
    MM2 = sb("MM2", [128, NTT, 32], F32)
    MALL = sb("MALL", [128, NTT * 32], BF16)
    GATES = sb("GATES", [128, NTT, 2], F32)
    POS = sb("POS", [128, NTT, 32], F32)
    TOTS = sb("TOTS", [128, NTT, 32], F32)
    CAR = sb("CAR", [128, NTT, 32], F32)
    OKM = sb("OKM", [128, NTT, 32], F32)
    TMP3 = sb("TMP3", [128, NTT, 32], F32)
    SF = sb("SF", [128, 4, NTT], F32)
    SI = sb("SI", [128, 2, NTT], I32)
    SID = [sb("SID%d" % i, [128, 16], I32) for i in range(4)]
    if ctx is None:
        banks = [st.enter_context(nc.psum_tensor("bk%d" % i, [128, 512], F32)) for i in range(8)]
        banks_b = [b.bitcast(BF16) for b in banks]
    else:
        banks = ctx["banks"]
        banks_b = ctx["banks_b"]
        MIDX = sb("MIDX", [128, 16 * NTT], I32)
    bbk = [Buf("bank%d" % i) for i in range(8)]
    bank_rr = [0]

    def next_bank():
        i = bank_rr[0]
        bank_rr[0] = (i + 1) % 8
        return i

    cp_rr = [0]

    def evac(out_ap, in_ap, reads, writes):
        cp_rr[0] ^= 1
        if cp_rr[0]:
            P.op("act", "activation", out_ap, in_ap, AF.Copy, reads=reads, writes=writes)
        else:
            P.op("dve", "tensor_copy", out_ap, in_ap, reads=reads, writes=writes)

    def fview(off_f32, n):
        return ARENAf[:, off_f32:off_f32 + n]

    def bview(off_f32, n):
        return ARENA[:, 2 * off_f32:2 * off_f32 + n]

    XR = [fview(0 + i * 2048, 2048) for i in range(2)]
    HH = [fview(4096 + i * 2048, 2048) for i in range(2)]
    X1 = [fview(8192 + i * 2048, 2048) for i in range(2)]
    XB = [bview(12288 + i * 1024, 2048) for i in range(2)]
    X1T = fview(14336, 2048).rearrange("p (c t) -> p c t", c=16)
    MT = [bview(16384 + i * 1024, 2048).rearrange("p (c t) -> p c t", c=16) for i in range(2)]
    YA = [fview(12288 + i * 2048, 2048) for i in range(2)]
    YB = [fview(16384 + i * 2048, 2048) for i in range(2)]
    WD = [bview(0 + i * 4096, 8192).rearrange("p (c n) -> p c n", c=4) for i in range(2)]
    XG = [bview(8192 + i * 1024, 2048) for i in range(NSL)]
    XeT = [bview(8192 + NSL * 1024 + i * (8 * C), 16 * C).rearrange("p (c s) -> p c s", c=16) for i in range(2)]
    o3 = 8192 + NSL * 1024 + 2 * 8 * C
    HS = fview(o3, C)
    HT = [bview(o3 + C + i * 2 * C, 4 * C).rearrange("p (c s) -> p c s", c=4) for i in range(2)]
    o4 = o3 + C + 4 * C
    YSB = [fview(o4 + i * 2048, 2048) for i in range(2)]
    assert o4 + 4096 <= 20480
    WO = BIGW[:, :].rearrange("p (c n) -> p c n", c=16)
    WG = [BIGW[:, i * 8192:(i + 1) * 8192].rearrange("p (c n) -> p c n", c=16) for i in range(2)]
    WU = [BIGW[:, 16384 + i * 8192:16384 + (i + 1) * 8192].rearrange("p (c n) -> p c n", c=16) for i in range(2)]

    bXR, bHH, bX1, bXB, bMT = ([Buf(), Buf()] for _ in range(5))
    bX1T, bLG, bSMB, bSTATS, bT8, bEM = Buf(), Buf(), Buf(), Buf(), Buf(), Buf()
    bWO, bLNP, bCONST, bROUT = Buf(), Buf(), Buf(), Buf()
    bX1F = [Buf() for _ in range(NTT)]
    bX1H = Buf()
    bST, bYS, bOUT = Buf(), Buf(), Buf()
    bDISP = Buf()

    P.dma("sp", bLNP, LNP[:, :], ln_d.partition_broadcast(128), writes=[bLNP])
    P.dma("sp", bCONST, BR[:, :], br_d.partition_broadcast(128), writes=[bCONST])
    P.dma("sp", bCONST, EC[:, :], ec_d.partition_broadcast(128), writes=[bCONST])
    P.dma("sp", bCONST, IDENT[:, :], ident_d, writes=[bCONST])
    P.dma("sp", bCONST, TRISf[:, :], tris_d, writes=[bCONST])
    P.dma("sp", bCONST, TOK[:, :], tok_d, writes=[bCONST])
    P.dma("sp", bCONST, WR[:, :, :], wr_d.rearrange("(c p) n -> p c n", p=128), writes=[bCONST])
    P.op("dve", "tensor_copy", IDENTb[:, :], IDENT[:, :], reads=[bCONST], writes=[bCONST])
    P.op("dve", "tensor_copy", TRISb[:, :], TRISf[:, :], reads=[bCONST], writes=[bCONST])
    P.op("dve", "memset", ONESb[:, :], 1.0, writes=[bCONST])
    P.op("dve", "memset", ZI[:, :], 0, writes=[bCONST])
    wo_v = wo_d.rearrange("(c p) n -> p c n", p=128)
    for c4 in range(8):
        P.dma("pool", bWO, WO[:, c4 * 2:(c4 + 1) * 2, :], wo_v[:, c4 * 2:(c4 + 1) * 2, :], writes=[bWO])
    P.dma("sp", bST, ST_d.rearrange("(p n) k -> p (n k)", p=128), ZI[:, :], reads=[bCONST], writes=[bST])

    bWSH, bWGA = Buf(), Buf()
    if ctx is not None:
        bWGA = ctx["bWGA"]
        bMIDX = Buf()
        P.dma("sp", bMIDX, MIDX[:, :], ctx["midx"], writes=[bMIDX])
        MTF = fview(18432, 2048).rearrange("p (c t) -> p c t", c=16)
        bMTF = Buf()
        gmt_rows = ctx["GMT"].rearrange("r (tb t) -> (r tb) t", t=128)
    for src_d, int_d in ((wgs_d, wgi_d), (wus_d, wui_d), (wds_d, wdi_d)) if gather else ():
        nr = src_d.shape[0] // 16
        for q in range(16):
            P.dma("sp", bWSH, int_d[q * nr:(q + 1) * nr, :], src_d[q * nr:(q + 1) * nr, :], writes=[bWSH])
    for int_d, g_d in ((wgi_d, wg_d), (wui_d, wu_d), (wdi_d, wd_d)) if gather else ():
        P.coll("pool", bWGA, "AllGather", ALU.bypass, [list(range(8))], [int_d], [g_d], reads=[bWSH], writes=[bWGA])

    if ctx is None:
        mT_v = mT_d.rearrange("(c p) t -> p c t", p=128)
    G1v, B1v, G2v, B2v = (LNP[:, k * 2048:(k + 1) * 2048] for k in range(4))

    def layer_norm(src, dst, gv, bv, srcbuf, dstbuf):
        for k in range(4):
            P.op("dve", "bn_stats", STATS[:, k * 6:(k + 1) * 6], src[:, k * 512:(k + 1) * 512],
                 reads=[srcbuf], writes=[bSTATS])
        P.op("dve", "bn_aggr", SMB[:, 0:2], STATS[:, :], reads=[bSTATS], writes=[bSMB])
        P.op("dve", "tensor_scalar", SMB[:, 2:3], SMB[:, 1:2], LN_EPS, None, ALU.add, reads=[bSMB], writes=[bSMB])
        P.op("act", "activation", SMB[:, 2:3], SMB[:, 2:3], AF.Sqrt, reads=[bSMB], writes=[bSMB])
        P.op("dve", "reciprocal", SMB[:, 3:4], SMB[:, 2:3], reads=[bSMB], writes=[bSMB])
        P.op("dve", "tensor_scalar", dst, src, SMB[:, 0:1], SMB[:, 3:4], ALU.subtract, ALU.mult,
             reads=[srcbuf, bSMB], writes=[dstbuf])
        P.op("pool", "tensor_tensor", dst, dst, gv, ALU.mult, reads=[dstbuf, bLNP], writes=[dstbuf])
        P.op("pool", "tensor_tensor", dst, dst, bv, ALU.add, reads=[dstbuf, bLNP], writes=[dstbuf])

    for i in range(NTT):
        k = i % 2
        if ctx is None:
            for c4 in range(4):
                P.dma("pool", bMT[k], MT[k][:, c4 * 4:(c4 + 1) * 4, :],
                      mT_v[:, c4 * 4:(c4 + 1) * 4, i * 128:(i + 1) * 128], writes=[bMT[k]])
        else:
            for c in range(16):
                P.dma("pool", bMTF, MTF[:, c, :], gmt_rows, meth="indirect_dma_start", out_offset=None,
                      in_offset=bass.IndirectOffsetOnAxis(ap=MIDX[:, c * NTT + i:c * NTT + i + 1], axis=0),
                      reads=[bMIDX, ctx["bGMT"]], writes=[bMTF])
            P.op("pool", "tensor_copy", MT[k], MTF, reads=[bMTF], writes=[bMT[k]])
        P.dma("sp", bXR[k], XR[k], x_d[i * 128:(i + 1) * 128, :], writes=[bXR[k]])
        for nb in range(4):
            bi = next_bank()
            for c in range(16):
                P.op("pe", "matmul", banks[bi][:, :], lhsT=MT[k][:, c, :], rhs=WO[:, c, nb * 512:(nb + 1) * 512],
                     start=(c == 0), stop=(c == 15), reads=[bMT[k], bWO], writes=[bbk[bi]])
            P.op("dve", "scalar_tensor_tensor", HH[k][:, nb * 512:(nb + 1) * 512], XR[k][:, nb * 512:(nb + 1) * 512],
                 ALPHA, banks[bi][:, :], ALU.mult, ALU.add, reads=[bXR[k], bbk[bi]], writes=[bHH[k]])
        layer_norm(HH[k], X1[k], G1v, B1v, bHH[k], bX1[k])
        P.dma("sp", bX1F[i], X1F_d[i * 128:(i + 1) * 128, :], X1[k], reads=[bX1[k]], writes=[bX1F[i]])
        P.op("act", "activation", XB[k], X1[k], AF.Copy, reads=[bX1[k]], writes=[bXB[k]])
        P.dma("sp", bX1H, X1H_d[i * 128:(i + 1) * 128, :], XB[k], reads=[bXB[k]], writes=[bX1H])
        for q in range(4):
            bi = next_bank()
            for cc in range(4):
                c = q * 4 + cc
                P.op("pe", "transpose", banks[bi][:, cc * 128:(cc + 1) * 128], X1[k][:, c * 128:(c + 1) * 128],
                     IDENT[:, :], reads=[bX1[k], bCONST], writes=[bbk[bi]])
            evac(X1T[:, q * 4:(q + 1) * 4, :], banks[bi][:, :].rearrange("p (c t) -> p c t", c=4),
                 [bbk[bi]], [bX1T])
        bi = next_bank()
        for c in range(16):
            P.op("pe", "matmul", banks[bi][:, 0:36], lhsT=X1T[:, c, :], rhs=WR[:, c, :],
                 start=(c == 0), stop=(c == 15), reads=[bX1T, bCONST], writes=[bbk[bi]])
        P.op("dve", "tensor_tensor", LG[:, :], banks[bi][:, 0:36], BR[:, :], ALU.add,
             reads=[bbk[bi], bCONST], writes=[bLG])
        P.op("dve", "reduce_max", SMB[:, 8:9], LG[:, 0:4], AX.X, reads=[bLG], writes=[bSMB])
        P.op("dve", "tensor_scalar", SMB[:, 9:10], SMB[:, 8:9], -1.0, None, ALU.mult, reads=[bSMB], writes=[bSMB])
        P.op("dve", "tensor_scalar", SMB[:, 16:20], LG[:, 0:4], SMB[:, 8:9], None, ALU.is_equal,
             reads=[bLG, bSMB], writes=[bSMB])
        P.op("act", "activation", SMB[:, 24:28], LG[:, 0:4], AF.Exp, bias=SMB[:, 9:10], scale=1.0,
             accum_out=SMB[:, 10:11], reads=[bLG, bSMB], writes=[bSMB])
        P.op("dve", "reciprocal", SMB[:, 11:12], SMB[:, 10:11], reads=[bSMB], writes=[bSMB])
        P.op("dve", "tensor_scalar", SMB[:, 20:24], SMB[:, 16:20], 1.0, 1e30, ALU.subtract, ALU.mult,
             reads=[bSMB], writes=[bSMB])
        for g in range(4):
            P.op("dve", "tensor_scalar", EM[:, g * 8:(g + 1) * 8], LG[:, 4 + g * 8:4 + (g + 1) * 8],
                 SMB[:, 20 + g:21 + g], None, ALU.add, reads=[bLG, bSMB], writes=[bEM])
        P.op("dve", "max", T8[:, :], EM[:, :], reads=[bEM], writes=[bT8])
        P.op("dve", "tensor_scalar", MM1[:, i, :], EM[:, :], T8[:, 0:1], None, ALU.is_equal,
             reads=[bEM, bT8], writes=[bROUT])
        P.op("dve", "tensor_scalar", MM2[:, i, :], EM[:, :], T8[:, 1:2], None, ALU.is_equal,
             reads=[bEM, bT8], writes=[bROUT])
        P.op("dve", "tensor_tensor", MALL[:, i * 32:(i + 1) * 32], MM1[:, i, :], MM2[:, i, :], ALU.add,
             reads=[bROUT], writes=[bROUT])
        P.op("dve", "tensor_tensor", SMB[:, 12:13], T8[:, 1:2], T8[:, 0:1], ALU.subtract,
             reads=[bT8, bSMB], writes=[bSMB])
        P.op("act", "activation", SMB[:, 13:14], SMB[:, 12:13], AF.Exp, reads=[bSMB], writes=[bSMB])
        P.op("dve", "tensor_scalar", SMB[:, 14:15], SMB[:, 13:14], 1.0, None, ALU.add, reads=[bSMB], writes=[bSMB])
        P.op("dve", "reciprocal", SMB[:, 14:15], SMB[:, 14:15], reads=[bSMB], writes=[bSMB])
        P.op("dve", "tensor_tensor", SMB[:, 15:16], SMB[:, 13:14], SMB[:, 14:15], ALU.mult,
             reads=[bSMB], writes=[bSMB])
        P.op("dve", "tensor_scalar", GATES[:, i, :], SMB[:, 14:16], SMB[:, 11:12], None, ALU.mult,
             reads=[bSMB], writes=[bROUT])

    def early():
        P.op("dve", "memset", LNP[:, 0:2048], 0.0, reads=[bLNP], writes=[bLNP])
        P.dma("sp", bOUT, y_d[0:128, :], LNP[:, 0:2048], reads=[bLNP], writes=[bOUT])
        P.final_wait("sp", [bOUT])
        P.emit(nc, st)
        st.close()
        return nc
    if stage == 1:
        return early()
    bi = next_bank()
    P.op("pe", "matmul", banks[bi][:, 0:NTT * 32], lhsT=TRISb[:, :], rhs=MALL[:, :], start=True, stop=True,
         reads=[bROUT, bCONST], writes=[bbk[bi]])
    P.op("dve", "tensor_copy", POS[:, :, :], banks[bi][:, 0:NTT * 32].rearrange("p (t e) -> p t e", e=32),
         reads=[bbk[bi]], writes=[bDISP])
    bi = next_bank()
    P.op("pe", "matmul", banks[bi][:, 0:NTT * 32], lhsT=ONESb[:, :], rhs=MALL[:, :], start=True, stop=True,
         reads=[bROUT, bCONST], writes=[bbk[bi]])
    P.op("dve", "tensor_copy", TOTS[:, :, :], banks[bi][:, 0:NTT * 32].rearrange("p (t e) -> p t e", e=32),
         reads=[bbk[bi]], writes=[bDISP])
    P.op("dve", "memset", CAR[:, 0, :], 0.0, writes=[bDISP])
    for i in range(1, NTT):
        P.op("dve", "tensor_tensor", CAR[:, i, :], CAR[:, i - 1, :], TOTS[:, i - 1, :], ALU.add,
             reads=[bDISP], writes=[bDISP])
    P.op("dve", "tensor_tensor", POS[:, :, :], POS[:, :, :], CAR[:, :, :], ALU.add, reads=[bDISP], writes=[bDISP])
    P.op("dve", "tensor_scalar", OKM[:, :, :], POS[:, :, :], float(C), None, ALU.is_lt, reads=[bDISP], writes=[bDISP])
    for i in range(NTT):
        P.op("dve", "tensor_tensor", POS[:, i, :], POS[:, i, :], EC[:, :], ALU.add,
             reads=[bDISP, bCONST], writes=[bDISP])
    for j, MMj in enumerate((MM1, MM2)):
        P.op("dve", "tensor_tensor", TMP3[:, :, :], MMj[:, :, :], OKM[:, :, :], ALU.mult,
             reads=[bROUT, bDISP], writes=[bDISP])
        P.op("dve", "reduce_sum", SF[:, 2 + j, :], TMP3[:, :, :], AX.X, reads=[bDISP], writes=[bDISP])
        P.op("dve", "tensor_tensor", TMP3[:, :, :], TMP3[:, :, :], POS[:, :, :], ALU.mult,
             reads=[bDISP], writes=[bDISP])
        P.op("dve", "reduce_sum", SF[:, j, :], TMP3[:, :, :], AX.X, reads=[bDISP], writes=[bDISP])
        P.op("dve", "tensor_scalar", TMP3[:, 0, 0:NTT], SF[:, 2 + j, :], -float(NSLOT), float(NSLOT),
             ALU.mult, ALU.add, reads=[bDISP], writes=[bDISP])
        P.op("dve", "tensor_tensor", SF[:, j, :], SF[:, j, :], TMP3[:, 0, 0:NTT], ALU.add,
             reads=[bDISP], writes=[bDISP])
        P.op("dve", "tensor_copy", SI[:, j, :], SF[:, j, :], reads=[bDISP], writes=[bDISP])
        P.op("dve", "tensor_tensor", GATES[:, :, j], GATES[:, :, j], SF[:, 2 + j, :], ALU.mult,
             reads=[bDISP, bROUT], writes=[bROUT])
    for i in range(NTT):
        for j in range(2):
            P.dma("pool", bST, ST_d[:, :], TOK[:, i * 16:(i + 1) * 16], meth="indirect_dma_start",
                  out_offset=bass.IndirectOffsetOnAxis(ap=SI[:, j, i:i + 1], axis=0), in_offset=None,
                  reads=[bDISP, bCONST], writes=[bST])

    if stage == 2:
        return early()
    _barrier(P)
    P.op("dve", "memset", YSB[0], 0.0, writes=[bDISP])
    P.dma("sp", bYS, YS_d[NSLOT:NSLOT + 128, :], YSB[0], reads=[bDISP], writes=[bYS])
    bWG, bWU, bWD = ([Buf(), Buf()] for _ in range(3))
    bXG = [Buf() for _ in range(NSL)]
    bXeT, bHT, bYSB = ([Buf(), Buf()] for _ in range(3))
    bYSB[0] = bDISP
    bHS = Buf()
    bSID = [Buf() for _ in range(4)]
    sid_rr = 0
    wg_v = wg_d.rearrange("(e c p) n -> e p c n", p=128, c=16)
    wu_v = wu_d.rearrange("(e c p) n -> e p c n", p=128, c=16)
    wd_v = wd_d.rearrange("(e c p) n -> e p c n", p=128, c=4)
    ysb_rr = 0
    def load_expert(e):
        k = e % 2
        for c4 in range(4):
            P.dma("pool", bWG[k], WG[k][:, c4 * 4:(c4 + 1) * 4, :], wg_v[e, :, c4 * 4:(c4 + 1) * 4, :], reads=[bWGA], writes=[bWG[k]])
        for c4 in range(4):
            P.dma("pool", bWU[k], WU[k][:, c4 * 4:(c4 + 1) * 4, :], wu_v[e, :, c4 * 4:(c4 + 1) * 4, :], reads=[bWGA], writes=[bWU[k]])
        for c4 in range(2):
            P.dma("pool", bWD[k], WD[k][:, c4 * 2:(c4 + 1) * 2, :], wd_v[e, :, c4 * 2:(c4 + 1) * 2, :], reads=[bWGA], writes=[bWD[k]])

    load_expert(0)
    for e in range(32):
        k = e % 2
        if e + 1 < 32:
            load_expert(e + 1)
        for s in range(NSL):
            q = sid_rr % 4
            sid_rr += 1
            P.dma("sp", bSID[q], SID[q][:, :], ST_d[e * C + s * 128:e * C + (s + 1) * 128, :],
                  reads=[bST], writes=[bSID[q]])
            P.dma("pool", bXG[s], XG[s], X1H_d[:, :], meth="indirect_dma_start", out_offset=None,
                  in_offset=bass.IndirectOffsetOnAxis(ap=SID[q][:, 0:1], axis=0),
                  reads=[bSID[q], bX1H], writes=[bXG[s]])
            for hh in range(2):
                bi = next_bank()
                for cc in range(8):
                    c = hh * 8 + cc
                    P.op("pe", "transpose", banks_b[bi][:, cc * 128:(cc + 1) * 128], XG[s][:, c * 128:(c + 1) * 128],
                         IDENTb[:, :], reads=[bXG[s], bCONST], writes=[bbk[bi]])
                evac(XeT[k][:, hh * 8:(hh + 1) * 8, s * 128:(s + 1) * 128],
                     banks_b[bi][:, :].rearrange("p (c t) -> p c t", c=8), [bbk[bi]], [bXeT[k]])
        for dc in range(4):
            bg = next_bank()
            for c in range(16):
                P.op("pe", "matmul", banks[bg][:, 0:C], lhsT=WG[k][:, c, dc * 128:(dc + 1) * 128], rhs=XeT[k][:, c, :],
                     start=(c == 0), stop=(c == 15), reads=[bWG[k], bXeT[k]], writes=[bbk[bg]])
            bu = next_bank()
            for c in range(16):
                P.op("pe", "matmul", banks[bu][:, 0:C], lhsT=WU[k][:, c, dc * 128:(dc + 1) * 128], rhs=XeT[k][:, c, :],
                     start=(c == 0), stop=(c == 15), reads=[bWU[k], bXeT[k]], writes=[bbk[bu]])
            P.op("act", "activation", HS, banks[bg][:, 0:C], AF.Silu, reads=[bbk[bg]], writes=[bHS])
            P.op("dve", "tensor_tensor", HT[k][:, dc, :], HS, banks[bu][:, 0:C], ALU.mult,
                 reads=[bHS, bbk[bu]], writes=[bHT[k]])
        for s in range(NSL):
            yk = ysb_rr % 2
            ysb_rr += 1
            for nb in range(4):
                bi = next_bank()
                for dc in range(4):
                    P.op("pe", "matmul", banks[bi][:, :], lhsT=HT[k][:, dc, s * 128:(s + 1) * 128],
                         rhs=WD[k][:, dc, nb * 512:(nb + 1) * 512], start=(dc == 0), stop=(dc == 3),
                         reads=[bHT[k], bWD[k]], writes=[bbk[bi]])
                evac(YSB[yk][:, nb * 512:(nb + 1) * 512], banks[bi][:, :], [bbk[bi]], [bYSB[yk]])
            P.dma("sp", bYS, YS_d[e * C + s * 128:e * C + (s + 1) * 128, :], YSB[yk], reads=[bYSB[yk]], writes=[bYS])

    if stage == 3:
        return early()
    _barrier(P)
    bYA, bYB = [Buf(), Buf()], [Buf(), Buf()]
    bXR, bHH, bX1 = ([Buf(), Buf()] for _ in range(3))
    for i in range(NTT):
        k = i % 2
        P.dma("sp", bXR[k], XR[k], X1F_d[i * 128:(i + 1) * 128, :], writes=[bXR[k]])
        P.dma("pool", bYA[k], YA[k], YS_d[:, :], meth="indirect_dma_start", out_offset=None,
              in_offset=bass.IndirectOffsetOnAxis(ap=SI[:, 0, i:i + 1], axis=0), writes=[bYA[k]])
        P.dma("pool", bYB[k], YB[k], YS_d[:, :], meth="indirect_dma_start", out_offset=None,
              in_offset=bass.IndirectOffsetOnAxis(ap=SI[:, 1, i:i + 1], axis=0), writes=[bYB[k]])
        P.op("dve", "tensor_scalar", YA[k], YA[k], GATES[:, i, 0:1], None, ALU.mult, reads=[bYA[k]], writes=[bYA[k]])
        P.op("dve", "scalar_tensor_tensor", YA[k], YB[k], GATES[:, i, 1:2], YA[k], ALU.mult, ALU.add,
             reads=[bYB[k], bYA[k]], writes=[bYA[k]])
        P.op("dve", "scalar_tensor_tensor", HH[k], XR[k], ALPHA, YA[k], ALU.mult, ALU.add,
             reads=[bXR[k], bYA[k]], writes=[bHH[k]])
        layer_norm(HH[k], X1[k], G2v, B2v, bHH[k], bX1[k])
        P.dma("sp", bOUT, y_d[i * 128:(i + 1) * 128, :], X1[k], reads=[bX1[k]], writes=[bOUT])
    if ctx is not None:
        ctx["bY"] = bOUT
        return None
    P.final_wait("sp", [bOUT])
    P.emit(nc, st)
    st.close()
    return nc


def b_in_maps(mergedT, x_tok, wo, ln1g, ln1b, ln2g, ln2b, wrg, brg, wre, bre, wg, wu, wd, T, C):
    NTT = T // 128
    ln = np.concatenate([ln1g, ln1b, ln2g, ln2b]).astype(np.float32)[None]
    wr = np.ascontiguousarray(np.concatenate([wrg, wre], axis=1))
    br = np.concatenate([brg, bre]).astype(np.float32)[None]
    ident = np.eye(128, dtype=np.float32)
    tris = np.triu(np.ones((128, 128), np.float32), 1)
    ec = (np.arange(32, dtype=np.float32) * C)[None]
    tok = (np.arange(NTT)[None, :, None] * 128 + np.arange(128)[:, None, None] + np.zeros((1, 1, 16))).astype(np.int32)
    tok = np.ascontiguousarray(tok.reshape(128, NTT * 16))
    maps = []
    for c in range(8):
        maps.append(dict(mT=np.ascontiguousarray(mergedT[:, c * T:(c + 1) * T]),
                         x=np.ascontiguousarray(x_tok[c * T:(c + 1) * T]),
                         wo=wo, ln=ln, wr=wr, br=br,
                         wg=np.ascontiguousarray(wg[4 * c:4 * c + 4]).reshape(4 * 2048, 512),
                         wu=np.ascontiguousarray(wu[4 * c:4 * c + 4]).reshape(4 * 2048, 512),
                         wd=np.ascontiguousarray(wd[4 * c:4 * c + 4]).reshape(4 * 512, 2048),
                         ident=ident, tris=tris, ec=ec, tok=tok))
    return maps


class _Arena:
    def __init__(self, nc, stack, nbytes):
        self.t16 = stack.enter_context(nc.sbuf_tensor("ARENA_ALL", [128, nbytes // 2], BF16))
        self.t32 = self.t16.bitcast(F32)
        self.ti32 = self.t16.bitcast(I32)
        self.nbytes = nbytes
        self.off = 0
        self.where = {}

    def reset(self):
        self.off = 0
        self.where = {}

    def _view(self, off, shape, dt):
        size = 2 if dt == BF16 else 4
        base = {BF16: self.t16, F32: self.t32, I32: self.ti32}[dt]
        n = 1
        for s in shape[1:]:
            n *= s
        v = base[:, off // size:off // size + n]
        if len(shape) == 3:
            v = v.rearrange("p (a b) -> p a b", a=shape[1])
        elif len(shape) == 4:
            v = v.rearrange("p (a b c) -> p a b c", a=shape[1], b=shape[2])
        return v, n * size

    def alloc(self, name, shape, dt):
        off = (self.off + 63) // 64 * 64
        v, nb = self._view(off, shape, dt)
        self.off = off + nb
        assert self.off <= self.nbytes, (name, self.off, self.nbytes)
        self.where[id(v)] = off
        return v

    def alias_f32(self, ap, n):
        off = self.where[id(ap)]
        v, _ = self._view(off, [128, n], F32)
        return v


def build_L(S, C, last_wait=True, arena_kb=204):
    from contextlib import ExitStack
    T = S // 4
    NTT = T // 128
    NSLOT = 32 * C
    NROW = NSLOT + 128
    nc = bass.Bass("TRN2", target_bir_lowering=False)
    dt_in = lambda name, shape, dt=F32: nc.dram_tensor(name, shape, dt, kind="ExternalInput").ap()
    ctx = dict(nc=nc)
    ctx["xT"] = dt_in("xT", [2048, S])
    ctx["x"] = dt_in("x", [T, 2048])
    ctx["wc"] = dt_in("wc", [2048, NWC])
    ctx["cc"] = dt_in("cc", [32, S])
    ctx["ss"] = dt_in("ss", [32, S])
    ctx["misc"] = dt_in("misc", [1, MISC])
    ctx["tri"] = dt_in("tri", [128, 128])
    ctx["mask"] = dt_in("mask", [128, 128])
    ctx["wo"] = dt_in("wo", [2048, 2048])
    ctx["ln"] = dt_in("ln", [1, 8192])
    ctx["wr"] = dt_in("wr", [2048, 36])
    ctx["br"] = dt_in("br", [1, 36])
    wgs_d = dt_in("wg", [4 * 2048, 512])
    wus_d = dt_in("wu", [4 * 2048, 512])
    wds_d = dt_in("wd", [4 * 512, 2048])
    ctx["ident"] = dt_in("ident", [128, 128])
    ctx["tris"] = dt_in("tris", [128, 128])
    ctx["ec"] = dt_in("ec", [1, 32])
    ctx["tok"] = dt_in("tok", [128, NTT * 16], I32)
    ctx["midx"] = dt_in("midx", [128, 16 * NTT], I32)
    ctx["y"] = nc.dram_tensor("y", [T, 2048], F32, kind="ExternalOutput").ap()
    it = lambda name, shape, dt=F32, **kw: nc.dram_tensor(name, shape, dt, kind="Internal", **kw).ap()
    wgi_d = it("wgi", [4 * 2048, 512])
    wui_d = it("wui", [4 * 2048, 512])
    wdi_d = it("wdi", [4 * 512, 2048])
    ctx["wgg"] = it("wgg", [32 * 2048, 512], addr_space="Local")
    ctx["wug"] = it("wug", [32 * 2048, 512], addr_space="Local")
    ctx["wdg"] = it("wdg", [32 * 512, 2048], addr_space="Local")
    ctx["MTint"] = it("MTint", [512, S])
    ctx["GMT"] = it("GMT", [4 * 512, S], addr_space="Local")
    ctx["X1F"] = it("X1F", [T, 2048])
    ctx["X1H"] = it("X1H", [T, 2048], BF16)
    ctx["ST"] = it("ST", [NROW, 16], I32)
    ctx["YS"] = it("YS", [NROW, 2048])

    P = Prog()
    st = ExitStack()
    arena = _Arena(nc, st, arena_kb * 1024)
    ctx["P"] = P
    ctx["alloc"] = arena.alloc
    ctx["alias_f32"] = arena.alias_f32
    banks = [st.enter_context(nc.psum_tensor("bk%d" % i, [128, 512], F32)) for i in range(8)]
    ctx["banks"] = banks
    ctx["banks_b"] = [b.bitcast(BF16) for b in banks]

    bWSH, bWGA = Buf(), Buf()
    for src_d, int_d in ((wgs_d, wgi_d), (wus_d, wui_d), (wds_d, wdi_d)):
        nr = src_d.shape[0] // 16
        for q in range(16):
            P.dma("sp", bWSH, int_d[q * nr:(q + 1) * nr, :], src_d[q * nr:(q + 1) * nr, :], writes=[bWSH])
    for int_d, g_d in ((wgi_d, ctx["wgg"]), (wui_d, ctx["wug"]), (wdi_d, ctx["wdg"])):
        P.coll("pool", bWGA, "AllGather", ALU.bypass, [list(range(8))], [int_d], [g_d], reads=[bWSH], writes=[bWGA])
    ctx["bWGA"] = bWGA
    _barrier(P)

    build_A(S, ctx=ctx)
    bGMT = Buf()
    P.coll("pool", bGMT, "AllGather", ALU.bypass, [[0, 1, 2, 3], [4, 5, 6, 7]], [ctx["MTint"]], [ctx["GMT"]],
           reads=[ctx["bMT"]], writes=[bGMT])
    ctx["bGMT"] = bGMT
    _barrier(P)
    arena.reset()
    build_B(T, C, ctx=ctx)
    P.final_wait("sp", [ctx["bY"]])
    P.emit(nc, st)
    st.close()
    return nc


def wo_perm_rows():
    rows = []
    for r in range(4):
        for lc in range(512):
            rows.append(256 * r + lc if lc < 256 else 1024 + 256 * r + (lc - 256))
    return np.array(rows)


def l_in_maps(xT_b, x_tok, l, S, C, w_in_l, b_f_l, lam_l, subg_l, wo, ln1g, ln1b, ln2g, ln2b, wrg, brg, wre, bre, wg, wu, wd):
    T = S // 4
    NTT = T // 128
    mapsA = a_in_maps(xT_b, w_in_l, b_f_l, lam_l, subg_l, l, S)
    ln = np.concatenate([ln1g, ln1b, ln2g, ln2b]).astype(np.float32)[None]
    wr = np.ascontiguousarray(np.concatenate([wrg, wre], axis=1))
    br = np.concatenate([brg, bre]).astype(np.float32)[None]
    ident = np.eye(128, dtype=np.float32)
    tris = np.triu(np.ones((128, 128), np.float32), 1)
    ec = (np.arange(32, dtype=np.float32) * C)[None]
    tok = (np.arange(NTT)[None, :, None] * 128 + np.arange(128)[:, None, None] + np.zeros((1, 1, 16))).astype(np.int32)
    tok = np.ascontiguousarray(tok.reshape(128, NTT * 16))
    wop = np.ascontiguousarray(wo[wo_perm_rows()])
    maps = []
    p = np.arange(128)[:, None, None]
    cch = np.arange(16)[None, :, None]
    ii = np.arange(NTT)[None, None, :]
    for c in range(8):
        j = c % 4
        midx = (((cch // 4) * 512 + (cch % 4) * 128 + p) * (S // 128) + j * NTT + ii).astype(np.int32)
        m = dict(mapsA[c])
        m.update(x=np.ascontiguousarray(x_tok[c * T:(c + 1) * T]), wo=wop, ln=ln, wr=wr, br=br,
                 wg=np.ascontiguousarray(wg[4 * c:4 * c + 4]).reshape(4 * 2048, 512),
                 wu=np.ascontiguousarray(wu[4 * c:4 * c + 4]).reshape(4 * 2048, 512),
                 wd=np.ascontiguousarray(wd[4 * c:4 * c + 4]).reshape(4 * 512, 2048),
                 ident=ident, tris=tris, ec=ec, tok=tok,
                 midx=np.ascontiguousarray(midx.reshape(128, 16 * NTT)))
        maps.append(m)
    return maps


GSPACE_DEFAULT = "Local"
_NC_CACHE = {}


def _reset_backend():
    import jax
    jax.clear_caches()
    try:
        from jax._src import xla_bridge
        xla_bridge._clear_backends()
    except Exception:
        pass


def _get_nc(kind):
    if kind not in _NC_CACHE:
        _NC_CACHE[kind] = build_A(SEQ) if kind == "A" else build_B(2048, 256, gather=False)
    return _NC_CACHE[kind]


def kernel(x, w_in, b_f, diff_lambda, diff_subln_g, w_o, ln1_g, ln1_b, w_router_group,
           b_router_group, w_router_expert, b_router_expert, w_gate, w_up, w_down, ln2_g, ln2_b):
    x = np.asarray(x, np.float32)
    xt = x.reshape(BATCH * SEQ, D_MODEL)
    cores = list(range(8))
    for l in range(DEPTH):
        xT_b = [np.ascontiguousarray(xt[b * SEQ:(b + 1) * SEQ].T) for b in range(BATCH)]
        mapsA = a_in_maps(xT_b, np.asarray(w_in[l]), np.asarray(b_f[l]), np.asarray(diff_lambda[l]),
                          np.asarray(diff_subln_g[l]), l, SEQ)
        resA = run_bass_kernel_spmd(_get_nc("A"), mapsA, core_ids=cores)
        merged = a_assemble(resA.results, SEQ).reshape(BATCH * SEQ, D_MODEL)
        mapsB = b_in_maps(np.ascontiguousarray(merged.T), xt, np.asarray(w_o[l]),
                          np.asarray(ln1_g[l]), np.asarray(ln1_b[l]), np.asarray(ln2_g[l]), np.asarray(ln2_b[l]),
                          np.asarray(w_router_group[l]), np.asarray(b_router_group[l]),
                          np.asarray(w_router_expert[l]), np.asarray(b_router_expert[l]),
                          np.asarray(w_gate[l]), np.asarray(w_up[l]), np.asarray(w_down[l]), 2048, 256)
        wgf = np.asarray(w_gate[l]).reshape(32 * 2048, 512)
        wuf = np.asarray(w_up[l]).reshape(32 * 2048, 512)
        wdf = np.asarray(w_down[l]).reshape(32 * 512, 2048)
        for m in mapsB:
            for k in ("wg", "wu", "wd"):
                m.pop(k)
            m.update(wgg=wgf, wug=wuf, wdg=wdf)
        resB = run_bass_kernel_spmd(_get_nc("B"), mapsB, core_ids=cores)
        xt = np.concatenate([resB.results[c]["y"] for c in cores], axis=0)
    return xt.reshape(BATCH, SEQ, D_MODEL).astype(np.float32)
```
